# Optimizing a Trainium2 kernel written in Bass

```python
import jax, jax.numpy as jnp
from jax import lax
import numpy as np

D_MODEL = 4096
BATCH = 2
SEQ = 4096
DEPTH = 4

D_BRANCH = D_MODEL // 4
D_MIX = 4 * D_BRANCH
NORM_EPS = 1e-6

RET_HEADS = 4
RET_HEAD_DIM = D_BRANCH // RET_HEADS
RET_CHUNK = 256

MOBA_HEADS = 8
MOBA_HEAD_DIM = D_BRANCH // MOBA_HEADS
MOBA_BLOCK = 256
MOBA_TOPK = 3
MOBA_Q_BLOCK = 32

SSM_HEAD_DIM = 64
SSM_HEADS = D_BRANCH // SSM_HEAD_DIM
SSM_GROUPS = 4
SSM_HPG = SSM_HEADS // SSM_GROUPS
SSM_STATE = 128
SSM_CONV = 4
SSM_CHUNK = 256
SSM_CONV_DIM = D_BRANCH + 2 * SSM_GROUPS * SSM_STATE

LRU_BLOCKS = 8
LRU_BLOCK_DIM = D_BRANCH // LRU_BLOCKS
LRU_CONV = 4
LRU_C = 8.0

IN_SIZES = (D_BRANCH, D_BRANCH, D_BRANCH, D_BRANCH,
            D_BRANCH, D_BRANCH, D_BRANCH, D_BRANCH,
            D_BRANCH, SSM_CONV_DIM, SSM_HEADS,
            D_BRANCH, D_BRANCH)
D_IN = 8 * D_BRANCH + D_BRANCH + SSM_CONV_DIM + SSM_HEADS + 2 * D_BRANCH

kernel_name = 'hymba_style_retention_moba_ssd_rglru_trunk'


def rms_norm(x, g):
    xf = x.astype(jnp.float32)
    y = xf * lax.rsqrt(jnp.mean(xf * xf, axis=-1, keepdims=True) + NORM_EPS)
    return (y * g.astype(jnp.float32)).astype(x.dtype)


def pad_seq(t, mult):
    pad = (-t.shape[1]) % mult
    return jnp.pad(t, [(0, 0), (0, pad)] + [(0, 0)] * (t.ndim - 2))


def causal_depthwise_conv(x, w, b):
    width = w.shape[0]
    s = x.shape[1]
    xp = jnp.pad(x, ((0, 0), (width - 1, 0), (0, 0)))
    y = b
    for k in range(width):
        y = y + xp[:, k:k + s] * w[k]
    return y


def retention(q, k, v, gn_w):
    b_, s, _ = q.shape
    H, dh, C = RET_HEADS, RET_HEAD_DIM, RET_CHUNK
    q, k, v = (pad_seq(t.astype(jnp.float32), C) for t in (q, k, v))
    sp = q.shape[1]
    n = sp // C

    def heads(t):
        return t.reshape(b_, n, C, H, dh).transpose(1, 0, 3, 2, 4)

    q, k, v = heads(q), heads(k) * (dh ** -0.5), heads(v)
    log_g = jnp.log1p(-jnp.exp2(-5.0 - jnp.arange(H, dtype=jnp.float32)))
    idx = jnp.arange(C, dtype=jnp.float32)
    diff = idx[:, None] - idx[None, :]
    causal = diff >= 0
    dmat = jnp.where(causal, jnp.exp(log_g[:, None, None] * jnp.where(causal, diff, 0.0)), 0.0)
    q_decay = jnp.exp(log_g[:, None] * (idx + 1.0))[None, :, :, None]
    k_decay = jnp.exp(log_g[:, None] * (C - 1.0 - idx))[None, :, :, None]
    chunk_decay = jnp.exp(log_g * C)[None, :, None, None]

    def step(state, qkv):
        qc, kc, vc = qkv
        inner = jnp.einsum('bhid,bhjd->bhij', qc, kc) * dmat
        y = jnp.einsum('bhij,bhjv->bhiv', inner, vc)
        y = y + jnp.einsum('bhid,bhdv->bhiv', qc, state) * q_decay
        state = chunk_decay * state + jnp.einsum('bhjd,bhjv->bhdv', kc * k_decay, vc)
        return state, y

    state0 = jnp.zeros((b_, H, dh, dh), jnp.float32)
    _, y = lax.scan(step, state0, (q, k, v))
    y = y.transpose(1, 0, 3, 2, 4).reshape(b_, sp, H, dh)[:, :s]
    y = rms_norm(y, gn_w)
    return y.reshape(b_, s, H * dh)


def moba_attention(q, k, v):
    b_, s, _ = q.shape
    H, dh, L, QB = MOBA_HEADS, MOBA_HEAD_DIM, MOBA_BLOCK, MOBA_Q_BLOCK
    scale = dh ** -0.5
    slopes = jnp.exp2(-8.0 * (jnp.arange(H, dtype=jnp.float32) + 1.0) / H)
    qh = q.astype(jnp.float32).reshape(b_, s, H, dh).transpose(0, 2, 1, 3)
    kh = pad_seq(k.astype(jnp.float32), L).reshape(b_, -1, H, dh).transpose(0, 2, 1, 3)
    vh = pad_seq(v.astype(jnp.float32), L).reshape(b_, -1, H, dh).transpose(0, 2, 1, 3)
    nb = kh.shape[2] // L
    ks = min(MOBA_TOPK, nb)
    kb = kh.reshape(b_, H, nb, L, dh)
    vb = vh.reshape(b_, H, nb, L, dh)

    k_mean = kb.mean(axis=3)
    gate = jnp.einsum('bhsd,bhnd->bhsn', qh, k_mean)
    q_blk = jnp.arange(s) // L
    past = jnp.arange(nb)[None, :] < q_blk[:, None]
    gate = jnp.where(past, gate, -jnp.inf)
    _, sel = lax.top_k(gate, ks)
    sel_valid = sel < q_blk[:, None]

    nq = s // QB
    qc = qh.reshape(b_, H, nq, QB, dh).transpose(2, 0, 1, 3, 4)
    selc = sel.reshape(b_, H, nq, QB, ks).transpose(2, 0, 1, 3, 4)
    validc = sel_valid.reshape(b_, H, nq, QB, ks).transpose(2, 0, 1, 3, 4)
    gather_blocks = jax.vmap(jax.vmap(lambda blocks, ids: blocks[ids]))
    offs = jnp.arange(L)

    def one_chunk(args):
        ci, qi, si, vi = args
        qpos = ci * QB + jnp.arange(QB)
        own = (ci * QB) // L
        ksel = gather_blocks(kb, si)
        vsel = gather_blocks(vb, si)
        kpos_sel = si[..., None] * L + offs
        dist_sel = (qpos[:, None, None] - kpos_sel).astype(jnp.float32)
        s_sel = jnp.einsum('bhqd,bhqkld->bhqkl', qi, ksel) * scale - slopes[:, None, None, None] * dist_sel
        s_sel = jnp.where(vi[..., None], s_sel, -jnp.inf)
        kown = lax.dynamic_slice_in_dim(kb, own, 1, axis=2)[:, :, 0]
        vown = lax.dynamic_slice_in_dim(vb, own, 1, axis=2)[:, :, 0]
        kpos_own = own * L + offs
        dist_own = (qpos[:, None] - kpos_own[None, :]).astype(jnp.float32)
        s_own = jnp.einsum('bhqd,bhld->bhql', qi, kown) * scale - slopes[:, None, None] * dist_own
        s_own = jnp.where(kpos_own[None, :] <= qpos[:, None], s_own, -jnp.inf)
        scores = jnp.concatenate([s_sel.reshape(b_, H, QB, ks * L), s_own], axis=-1)
        p = jax.nn.softmax(scores, axis=-1)
        p_sel = p[..., :ks * L].reshape(b_, H, QB, ks, L)
        out = jnp.einsum('bhqkl,bhqkld->bhqd', p_sel, vsel)
        out = out + jnp.einsum('bhql,bhld->bhqd', p[..., ks * L:], vown)
        return out

    o = lax.map(one_chunk, (jnp.arange(nq), qc, selc, validc))
    return o.transpose(1, 0, 3, 2, 4).reshape(b_, s, H * dh)


def mamba2_ssd(z, xbc, dt_raw, conv_w, conv_b, dt_bias, a_log, d_skip, norm_w):
    b_, s, _ = z.shape
    G, R, P, N, Q = SSM_GROUPS, SSM_HPG, SSM_HEAD_DIM, SSM_STATE, SSM_CHUNK
    xbc = jax.nn.silu(causal_depthwise_conv(xbc, conv_w, conv_b)).astype(jnp.float32)
    xs, bm, cm = jnp.split(xbc, [D_BRANCH, D_BRANCH + G * N], axis=-1)
    dt = jax.nn.softplus(dt_raw.astype(jnp.float32) + dt_bias.astype(jnp.float32))
    a = -jnp.exp(a_log.astype(jnp.float32)).reshape(G, R)
    xs = pad_seq(xs.reshape(b_, s, G, R, P), Q)
    bm = pad_seq(bm.reshape(b_, s, G, N), Q)
    cm = pad_seq(cm.reshape(b_, s, G, N), Q)
    dt = pad_seq(dt.reshape(b_, s, G, R), Q)
    sp = xs.shape[1]
    c = sp // Q
    xs = xs.reshape(b_, c, Q, G, R, P)
    bm = bm.reshape(b_, c, Q, G, N)
    cm = cm.reshape(b_, c, Q, G, N)
    dt = dt.reshape(b_, c, Q, G, R)
    da_cs = jnp.cumsum(dt * a, axis=2)

    tri = (jnp.arange(Q)[:, None] >= jnp.arange(Q)[None, :])[None, None, :, :, None, None]
    seg = da_cs[:, :, :, None] - da_cs[:, :, None]
    lmat = jnp.exp(jnp.where(tri, seg, -jnp.inf))
    cb = jnp.einsum('bcign,bcjgn->bcijg', cm, bm)
    wmat = cb[..., None] * lmat * dt[:, :, None]
    y_diag = jnp.einsum('bcijgr,bcjgrp->bcigrp', wmat, xs)

    decay_states = jnp.exp(da_cs[:, :, -1:] - da_cs)
    x_w = xs * (decay_states * dt)[..., None]
    states = jnp.einsum('bclgn,bclgrp->bcgrpn', bm, x_w)
    chunk_decay = jnp.exp(da_cs[:, :, -1])

    def step(h, inp):
        st, dec = inp
        return h * dec[..., None, None] + st, h

    h0 = jnp.zeros((b_, G, R, P, N), jnp.float32)
    _, h_prev = lax.scan(step, h0, (states.swapaxes(0, 1), chunk_decay.swapaxes(0, 1)))
    h_prev = h_prev.swapaxes(0, 1)
    y_off = jnp.einsum('bclgn,bcgrpn->bclgrp', cm, h_prev) * jnp.exp(da_cs)[..., None]
    y = y_diag + y_off + d_skip.astype(jnp.float32).reshape(G, R)[..., None] * xs
    y = y.reshape(b_, sp, D_BRANCH)[:, :s]
    y = y * jax.nn.silu(z.astype(jnp.float32))
    y = rms_norm(y.reshape(b_, s, G, D_BRANCH // G), norm_w.reshape(G, D_BRANCH // G))
    return y.reshape(b_, s, D_BRANCH)


def rg_lru(x, conv_w, conv_b, w_a, b_a, w_x, b_x, lam):
    b_, s, _ = x.shape
    xc = causal_depthwise_conv(x, conv_w, conv_b).astype(jnp.float32)
    xb = xc.reshape(b_, s, LRU_BLOCKS, LRU_BLOCK_DIM)
    r = jax.nn.sigmoid(jnp.einsum('bsnd,nde->bsne', xb, w_a.astype(jnp.float32)).reshape(b_, s, D_BRANCH) + b_a)
    i = jax.nn.sigmoid(jnp.einsum('bsnd,nde->bsne', xb, w_x.astype(jnp.float32)).reshape(b_, s, D_BRANCH) + b_x)
    log_a = -LRU_C * r * jax.nn.softplus(-lam.astype(jnp.float32))
    a = jnp.exp(log_a)
    u = jnp.sqrt(-jnp.expm1(2.0 * log_a)) * (i * xc)

    def combine(left, right):
        a1, b1 = left
        a2, b2 = right
        return a1 * a2, a2 * b1 + b2

    _, h = lax.associative_scan(combine, (a, u), axis=1)
    return h


def hybrid_layer(x, norm_w, w_in, ret_gn_w, ssm_conv_w, ssm_conv_b, ssm_dt_bias, ssm_a_log, ssm_d,
                 ssm_norm_w, lru_conv_w, lru_conv_b, lru_w_a, lru_b_a, lru_w_x, lru_b_x, lru_lambda, w_out):
    h = rms_norm(x, norm_w)
    proj = jnp.einsum('bsd,de->bse', h, w_in)
    (rq, rk, rv, rg, mq, mk, mv, mg, sz, sxbc, sdt, lx, lg) = jnp.split(
        proj, np.cumsum(IN_SIZES)[:-1].tolist(), axis=-1)
    dt_ = h.dtype
    y_ret = (retention(rq, rk, rv, ret_gn_w) * jax.nn.silu(rg.astype(jnp.float32))).astype(dt_)
    y_moba = (moba_attention(mq, mk, mv) * jax.nn.silu(mg.astype(jnp.float32))).astype(dt_)
    y_ssm = mamba2_ssd(sz, sxbc, sdt, ssm_conv_w, ssm_conv_b, ssm_dt_bias, ssm_a_log, ssm_d,
                       ssm_norm_w).astype(dt_)
    y_lru = (rg_lru(lx, lru_conv_w, lru_conv_b, lru_w_a, lru_b_a, lru_w_x, lru_b_x, lru_lambda)
             * jax.nn.silu(lg.astype(jnp.float32))).astype(dt_)
    y = jnp.concatenate([y_ret, y_moba, y_ssm, y_lru], axis=-1)
    return x + jnp.einsum('bse,ed->bsd', y, w_out)


def setup_inputs(seed: int = 0) -> dict:
    key = jax.random.key(seed)
    ks = jax.random.split(key, 24)
    f32 = jnp.float32
    nrm = lambda k, shape, sc: jax.random.normal(k, shape, f32) * sc
    x = nrm(ks[0], (BATCH, SEQ, D_MODEL), 1.0)
    norm_w = 1.0 + nrm(ks[1], (DEPTH, D_MODEL), 0.02)
    w_in = nrm(ks[2], (DEPTH, D_MODEL, D_IN), D_MODEL ** -0.5)
    ret_gn_w = 1.0 + nrm(ks[3], (DEPTH, RET_HEADS, RET_HEAD_DIM), 0.02)
    ssm_conv_w = nrm(ks[4], (DEPTH, SSM_CONV, SSM_CONV_DIM), SSM_CONV ** -0.5)
    ssm_conv_b = nrm(ks[5], (DEPTH, SSM_CONV_DIM), 0.02)
    dt0 = jnp.exp(jax.random.uniform(ks[6], (DEPTH, SSM_HEADS), f32, np.log(1e-3), np.log(1e-1)))
    ssm_dt_bias = dt0 + jnp.log(-jnp.expm1(-dt0))
    ssm_a_log = jnp.log(jax.random.uniform(ks[7], (DEPTH, SSM_HEADS), f32, 1.0, 16.0))
    ssm_d = 1.0 + nrm(ks[8], (DEPTH, SSM_HEADS), 0.1)
    ssm_norm_w = 1.0 + nrm(ks[9], (DEPTH, D_BRANCH), 0.02)
    lru_conv_w = nrm(ks[10], (DEPTH, LRU_CONV, D_BRANCH), LRU_CONV ** -0.5)
    lru_conv_b = nrm(ks[11], (DEPTH, D_BRANCH), 0.02)
    lru_w_a = nrm(ks[12], (DEPTH, LRU_BLOCKS, LRU_BLOCK_DIM, LRU_BLOCK_DIM), LRU_BLOCK_DIM ** -0.5)
    lru_b_a = nrm(ks[13], (DEPTH, D_BRANCH), 0.02)
    lru_w_x = nrm(ks[14], (DEPTH, LRU_BLOCKS, LRU_BLOCK_DIM, LRU_BLOCK_DIM), LRU_BLOCK_DIM ** -0.5)
    lru_b_x = nrm(ks[15], (DEPTH, D_BRANCH), 0.02)
    a_c = jax.random.uniform(ks[16], (DEPTH, D_BRANCH), f32, 0.9, 0.999)
    a_base = a_c ** (1.0 / LRU_C)
    lru_lambda = jnp.log(a_base) - jnp.log1p(-a_base)
    w_out = nrm(ks[17], (DEPTH, D_MIX, D_MODEL), D_MIX ** -0.5)
    final_norm_w = 1.0 + nrm(ks[18], (D_MODEL,), 0.02)
    return {'x': x, 'norm_w': norm_w, 'w_in': w_in, 'ret_gn_w': ret_gn_w,
            'ssm_conv_w': ssm_conv_w, 'ssm_conv_b': ssm_conv_b, 'ssm_dt_bias': ssm_dt_bias,
            'ssm_a_log': ssm_a_log, 'ssm_d': ssm_d, 'ssm_norm_w': ssm_norm_w,
            'lru_conv_w': lru_conv_w, 'lru_conv_b': lru_conv_b, 'lru_w_a': lru_w_a, 'lru_b_a': lru_b_a,
            'lru_w_x': lru_w_x, 'lru_b_x': lru_b_x, 'lru_lambda': lru_lambda,
            'w_out': w_out, 'final_norm_w': final_norm_w}


def reference(x, norm_w, w_in, ret_gn_w, ssm_conv_w, ssm_conv_b, ssm_dt_bias, ssm_a_log, ssm_d,
              ssm_norm_w, lru_conv_w, lru_conv_b, lru_w_a, lru_b_a, lru_w_x, lru_b_x, lru_lambda,
              w_out, final_norm_w):
    for l in range(DEPTH):
        x = hybrid_layer(x, norm_w[l], w_in[l], ret_gn_w[l], ssm_conv_w[l], ssm_conv_b[l],
                         ssm_dt_bias[l], ssm_a_log[l], ssm_d[l], ssm_norm_w[l], lru_conv_w[l],
                         lru_conv_b[l], lru_w_a[l], lru_b_a[l], lru_w_x[l], lru_b_x[l],
                         lru_lambda[l], w_out[l])
    return rms_norm(x, final_norm_w)
```

```python
import numpy as np
import ml_dtypes
from contextlib import ExitStack
import concourse.bass as bass
import concourse.mybir as mybir
from concourse.bass_utils import run_bass_kernel_spmd

F32 = mybir.dt.float32
BF16 = mybir.dt.bfloat16
AF = mybir.ActivationFunctionType
ALU = mybir.AluOpType
AX = mybir.AxisListType

D_MODEL = 4096
SEQ = 4096
BATCH = 2
DEPTH = 4
NCORES = 8
EPS = 1e-6
NEG = -30000.0

NFM = 2820
NTM = 512
NCOL = NFM + NTM
R_Q, R_K, R_G = 0, 256, 512
M_Q, M_K, M_G = 768, 1024, 1280
S_Z, S_X, S_B, S_C, S_DT = 1536, 1792, 2048, 2176, 2304
L_X, L_G = 2308, 2564


class Res:
    __slots__ = ("name", "w", "r")

    def __init__(self, name):
        self.name = name
        self.w = None
        self.r = {}


class TK:
    ENGS = ("pe", "act", "dve", "pool", "sp")

    def __init__(self, nc, es):
        self.nc = nc
        self.es = es
        self.h = {"pe": nc.tensor, "act": nc.scalar, "dve": nc.vector, "pool": nc.gpsimd, "sp": nc.sync}
        self.sem = {k: es.enter_context(nc.semaphore("sem_" + k)) for k in self.ENGS}
        self.cnt = {k: 0 for k in self.ENGS}
        self.pending = {k: False for k in self.ENGS}
        self.known = {k: {} for k in self.ENGS}
        self.ops = {k: [] for k in self.ENGS}
        self.dsem = {}
        self.dcnt = {}
        self.nps = 0

    def sb(self, name, shape, dt):
        return self.es.enter_context(self.nc.sbuf_tensor("sb_" + name, list(shape), dt))[:]

    def ps(self, name, shape=(128, 512), dt=F32):
        return self.es.enter_context(self.nc.psum_tensor("ps_" + name, list(shape), dt))[:]

    def _need(self, eng, tok):
        if tok is None:
            return
        kind, k, i = tok
        if kind == "e" and k == eng and eng == "pe":
            return
        if self.known[eng].get((kind, k), 0) >= i:
            return
        self.known[eng][(kind, k)] = i
        self.ops[eng].append(("wait", kind, k, i))

    def _deps(self, eng, reads, writes):
        for r in reads:
            self._need(eng, r.w)
        for r in writes:
            self._need(eng, r.w)
            for (kind, k), i in r.r.items():
                self._need(eng, (kind, k, i))

    def _mark(self, tok, reads, writes):
        for r in reads:
            key = (tok[0], tok[1])
            if r.r.get(key, 0) < tok[2]:
                r.r[key] = tok[2]
        for r in writes:
            r.w = tok
            r.r = {}

    def op(self, eng, fn, reads=(), writes=(), sig=True):
        self._deps(eng, reads, writes)
        idx = self.cnt[eng] + 1
        if sig:
            self.cnt[eng] = idx
            self.pending[eng] = False
        else:
            self.pending[eng] = True
        self.ops[eng].append(("op", fn, sig))
        self._mark(("e", eng, idx), reads, writes)

    def pe(self, fn, reads=(), writes=(), sig=True):
        self.op("pe", fn, reads, writes, sig)

    def act(self, fn, reads=(), writes=()):
        self.op("act", fn, reads, writes)

    def dve(self, fn, reads=(), writes=()):
        self.op("dve", fn, reads, writes)

    def pool(self, fn, reads=(), writes=()):
        self.op("pool", fn, reads, writes)

    def dma(self, q, out, in_, reads, writes, key):
        if key not in self.dsem:
            self.dsem[key] = self.es.enter_context(self.nc.semaphore("dsem_" + key))
            self.dcnt[key] = 0
        self._deps(q, reads, writes)
        self.dcnt[key] += 1
        self.ops[q].append(("dma", out, in_, key))
        self._mark(("d", key, self.dcnt[key]), reads, writes)

    def finish(self):
        for key, c in self.dcnt.items():
            self._need("sp", ("d", key, c))
        for e in self.ENGS:
            assert not self.pending[e], "engine %s ends with an unsignalled instruction" % e
        block = self.es.enter_context(self.nc.Block())

        def replay(name):
            def body(e):
                for o in self.ops[name]:
                    if o[0] == "wait":
                        _, kind, k, i = o
                        if kind == "e":
                            e.wait_ge(self.sem[k], i)
                        else:
                            e.wait_ge(self.dsem[k], 16 * i)
                    elif o[0] == "op":
                        ins = o[1](e)
                        if o[2]:
                            ins.then_inc(self.sem[name], 1)
                    else:
                        _, out, in_, key = o
                        e.dma_start(out=out, in_=in_).then_inc(self.dsem[key], 16)
            return body

        block.tensor(replay("pe"))
        block.scalar(replay("act"))
        block.vector(replay("dve"))
        block.gpsimd(replay("pool"))
        block.sync(replay("sp"))


class Buf:
    def __init__(self, tk, name, shape, dt, n=1):
        self.t = [tk.sb("%s%d" % (name, i), shape, dt) for i in range(n)]
        self.r = [Res("%s%d" % (name, i)) for i in range(n)]
        self.n = n
        self.i = -1
        self.name = name

    def next(self):
        self.i = (self.i + 1) % self.n
        return self.t[self.i], self.r[self.i], "%s%d" % (self.name, self.i)


class PsumPool:
    def __init__(self, tk, n, name="ps"):
        self.t = [tk.ps("%s%d" % (name, i)) for i in range(n)]
        self.r = [Res("%s%d" % (name, i)) for i in range(n)]
        self.n = n
        self.i = -1

    def next(self):
        self.i = (self.i + 1) % self.n
        return self.t[self.i], self.r[self.i]


def new_nc():
    return bass.Bass("TRN2", target_bir_lowering=False)


P_GROUPS = [
    (0, 768, "fm"), (768, 768, "fm"), (1536, 772, "fm"), (2308, 512, "fm"), (2820, 512, "tm"),
]


def build_P(S=SEQ, D=D_MODEL):
    nc = new_nc()
    KT = D // 128
    NTB = S // 512
    hT = nc.dram_tensor("hT", [128, KT * S], BF16, kind="ExternalInput").ap()
    w = nc.dram_tensor("w", [D, NCOL], F32, kind="ExternalInput").ap()
    fm = nc.dram_tensor("fm", [NFM, S], F32, kind="ExternalOutput").ap()
    tm = nc.dram_tensor("tm", [S, NTM], F32, kind="ExternalOutput").ap()
    hv = hT.rearrange("p (kt s) -> p kt s", kt=KT)
    wv = w.rearrange("(kt p) c -> p kt c", p=128)
    with ExitStack() as es:
        tk = TK(nc, es)
        wb = Buf(tk, "wb", [128, KT, 772], BF16, 2)
        hb = Buf(tk, "hb", [128, KT, 512], BF16, 2)
        stg = Buf(tk, "stg", [128, 512], F32, 4)
        pp = PsumPool(tk, 6)
        ev = 0
        def load_w(gi):
            c0, ncg, _ = P_GROUPS[gi]
            t, r, key = wb.next()
            for q4 in range(4):
                ks = slice(q4 * (KT // 4), (q4 + 1) * (KT // 4))
                tk.dma("pool", t[:, ks, 0:ncg], wv[:, ks, c0:c0 + ncg], [], [r], key)
            return t, r

        def load_h(tb):
            t, r, key = hb.next()
            tk.dma("sp", t[:, :, :], hv[:, :, tb * 512:(tb + 1) * 512], [], [r], key)
            return t, r

        wcur = load_w(0)
        hnext = load_h(0)
        for gi, (c0, ncg, kind) in enumerate(P_GROUPS):
            wt, wr = wcur
            for tb in range(NTB):
                ht, hr = hnext
                if not (gi == len(P_GROUPS) - 1 and tb == NTB - 1):
                    hnext = load_h((tb + 1) % NTB)
                if tb == 0 and gi + 1 < len(P_GROUPS):
                    wcur = load_w(gi + 1)
                if kind == "fm":
                    tiles = [(o, min(128, ncg - o)) for o in range(0, ncg, 128)]
                    for (o, m) in tiles:
                        pt, pr = pp.next()
                        for kt in range(KT):
                            tk.pe(lambda e, pt=pt, wt=wt, ht=ht, kt=kt, o=o, m=m: e.matmul(
                                pt[0:m, :], wt[:, kt, o:o + m], ht[:, kt, :], start=(kt == 0), stop=(kt == KT - 1)),
                                [wr, hr], [pr], sig=(kt == KT - 1))
                        st, sr, skey = stg.next()
                        if ev % 2 == 0:
                            tk.act(lambda e, st=st, pt=pt, m=m: e.activation(out=st[0:m, :], in_=pt[0:m, :], func=AF.Copy), [pr], [sr])
                        else:
                            tk.dve(lambda e, st=st, pt=pt, m=m: e.tensor_copy(out=st[0:m, :], in_=pt[0:m, :]), [pr], [sr])
                        ev += 1
                        tk.dma("sp", fm[c0 + o:c0 + o + m, tb * 512:(tb + 1) * 512], st[0:m, :], [sr], [], skey)
                else:
                    for ts in range(4):
                        pt, pr = pp.next()
                        for kt in range(KT):
                            tk.pe(lambda e, pt=pt, wt=wt, ht=ht, kt=kt, ts=ts, ncg=ncg: e.matmul(
                                pt[:, 0:ncg], ht[:, kt, ts * 128:(ts + 1) * 128], wt[:, kt, 0:ncg], start=(kt == 0), stop=(kt == KT - 1)),
                                [wr, hr], [pr], sig=(kt == KT - 1))
                        st, sr, skey = stg.next()
                        if ev % 2 == 0:
                            tk.act(lambda e, st=st, pt=pt: e.activation(out=st[:, :], in_=pt[:, :], func=AF.Copy), [pr], [sr])
                        else:
                            tk.dve(lambda e, st=st, pt=pt: e.tensor_copy(out=st[:, :], in_=pt[:, :]), [pr], [sr])
                        ev += 1
                        t0 = tb * 512 + ts * 128
                        tk.dma("sp", tm[t0:t0 + 128, c0 - NFM:c0 - NFM + ncg], st[:, 0:ncg], [sr], [], skey)
        tk.finish()
    return nc


def build_B(TB=1024, D=D_MODEL, proj=True, final=False):
    nc = new_nc()
    KT = D // 128
    DT = D // 128
    NB = TB // 512
    xT = nc.dram_tensor("xT", [D, TB], F32, kind="ExternalInput").ap()
    nw = nc.dram_tensor("nw", [128, DT], F32, kind="ExternalInput").ap()
    if proj:
        yT = nc.dram_tensor("yT", [128, KT * TB], BF16, kind="ExternalInput").ap()
        wo = nc.dram_tensor("wo", [D, D], F32, kind="ExternalInput").ap()
        yv = yT.rearrange("p (kt s) -> p kt s", kt=KT)
        wv = wo.rearrange("(kt p) c -> p kt c", p=128)
        xn = nc.dram_tensor("xn", [D, TB], F32, kind="ExternalOutput").ap()
    else:
        xn = xT
    hn = nc.dram_tensor("hn", [D, TB], F32 if final else BF16, kind="ExternalOutput").ap()
    with ExitStack() as es:
        tk = TK(nc, es)
        nwt = tk.sb("nwt", [128, DT], F32)
        nwr = Res("nw")
        tk.dma("sp", nwt[:, :], nw, [], [nwr], "const")
        ones = tk.sb("ones", [128, 128], F32)
        onesr = Res("ones")
        tk.dve(lambda e: e.memset(ones[:, :], 1.0), [], [onesr])
        epst = tk.sb("epst", [128, 1], F32)
        epsr = Res("eps")
        tk.dve(lambda e: e.memset(epst[:, :], EPS), [], [epsr])
        acc = tk.sb("acc", [128, TB], F32)
        accr = Res("acc")
        tk.pool(lambda e: e.memset(acc[:, :], 0.0), [], [accr])
        rstd = tk.sb("rstd", [128, TB], F32)
        rstdr = Res("rstd")
        xin = Buf(tk, "xin", [128, 512], F32, 3)
        xo = Buf(tk, "xo", [128, 512], F32, 3)
        sq = Buf(tk, "sq", [128, 512], F32, 2)
        pp = PsumPool(tk, 6)
        xnres = {}
        if proj:
            yb = tk.sb("yb", [128, KT, TB], BF16)
            ybr = Res("yb")
            for q4 in range(4):
                ks = slice(q4 * (KT // 4), (q4 + 1) * (KT // 4))
                tk.dma("sp", yb[:, ks, :], yv[:, ks, :], [], [ybr], "yb")
            wb = Buf(tk, "wb", [128, KT, 512], BF16, 2)
            NG = D // 512

            def load_w(gi):
                t, r, key = wb.next()
                for q4 in range(4):
                    ks = slice(q4 * (KT // 4), (q4 + 1) * (KT // 4))
                    tk.dma("pool", t[:, ks, :], wv[:, ks, gi * 512:(gi + 1) * 512], [], [r], key)
                return t, r
            wcur = load_w(0)
            for gi in range(NG):
                wt, wr = wcur
                if gi + 1 < NG:
                    wcur = load_w(gi + 1)
                for dl in range(4):
                    dt_ = gi * 4 + dl
                    for tb in range(NB):
                        xt, xr, xkey = xin.next()
                        tk.dma("sp", xt[:, :], xT[dt_ * 128:(dt_ + 1) * 128, tb * 512:(tb + 1) * 512], [], [xr], xkey)
                        pt, pr = pp.next()
                        for kt in range(KT):
                            tk.pe(lambda e, pt=pt, wt=wt, kt=kt, dl=dl, tb=tb: e.matmul(
                                pt[:, :], wt[:, kt, dl * 128:(dl + 1) * 128], yb[:, kt, tb * 512:(tb + 1) * 512],
                                start=(kt == 0), stop=(kt == KT - 1)), [wr, ybr], [pr], sig=(kt == KT - 1))
                        ot, orr, okey = xo.next()
                        tk.dve(lambda e, ot=ot, pt=pt, xt=xt: e.tensor_tensor(out=ot[:, :], in0=pt[:, :], in1=xt[:, :], op=ALU.add), [pr, xr], [orr])
                        dr = Res("xn_%d_%d" % (dt_, tb))
                        xnres[(dt_, tb)] = dr
                        tk.dma("sp", xn[dt_ * 128:(dt_ + 1) * 128, tb * 512:(tb + 1) * 512], ot[:, :], [orr], [dr], okey)
                        st, sr, _ = sq.next()
                        tk.act(lambda e, st=st, ot=ot: e.activation(out=st[:, :], in_=ot[:, :], func=AF.Square), [orr], [sr])
                        tk.pool(lambda e, st=st, tb=tb: e.tensor_tensor(out=acc[:, tb * 512:(tb + 1) * 512], in0=acc[:, tb * 512:(tb + 1) * 512], in1=st[:, :], op=ALU.add), [sr, accr], [accr])
        else:
            for dt_ in range(DT):
                for tb in range(NB):
                    xt, xr, xkey = xin.next()
                    tk.dma("sp", xt[:, :], xT[dt_ * 128:(dt_ + 1) * 128, tb * 512:(tb + 1) * 512], [], [xr], xkey)
                    st, sr, _ = sq.next()
                    tk.act(lambda e, st=st, xt=xt: e.activation(out=st[:, :], in_=xt[:, :], func=AF.Square), [xr], [sr])
                    tk.pool(lambda e, st=st, tb=tb: e.tensor_tensor(out=acc[:, tb * 512:(tb + 1) * 512], in0=acc[:, tb * 512:(tb + 1) * 512], in1=st[:, :], op=ALU.add), [sr, accr], [accr])
                    xnres[(dt_, tb)] = Res("xn_%d_%d" % (dt_, tb))
        for tb in range(NB):
            pt, pr = pp.next()
            tk.pe(lambda e, pt=pt, tb=tb: e.matmul(pt[:, :], ones[:, :], acc[:, tb * 512:(tb + 1) * 512], start=True, stop=True), [onesr, accr], [pr])
            tk.act(lambda e, pt=pt, tb=tb: e.activation(out=rstd[:, tb * 512:(tb + 1) * 512], in_=pt[:, :], func=AF.Sqrt, scale=1.0 / D, bias=epst[:, 0:1]), [pr, epsr], [rstdr])
            tk.dve(lambda e, tb=tb: e.reciprocal(out=rstd[:, tb * 512:(tb + 1) * 512], in_=rstd[:, tb * 512:(tb + 1) * 512]), [rstdr], [rstdr])
        ho = Buf(tk, "ho", [128, 512], F32 if final else BF16, 3)
        for dt_ in range(DT):
            for tb in range(NB):
                xt, xr, xkey = xin.next()
                tk.dma("sp", xt[:, :], xn[dt_ * 128:(dt_ + 1) * 128, tb * 512:(tb + 1) * 512], [xnres[(dt_, tb)]], [xr], xkey)
                ot, orr, okey = ho.next()
                tk.dve(lambda e, ot=ot, xt=xt, dt_=dt_, tb=tb: e.scalar_tensor_tensor(
                    out=ot[:, :], in0=xt[:, :], scalar=nwt[:, dt_:dt_ + 1], in1=rstd[:, tb * 512:(tb + 1) * 512], op0=ALU.mult, op1=ALU.mult),
                    [xr, nwr, rstdr], [orr])
                tk.dma("sp", hn[dt_ * 128:(dt_ + 1) * 128, tb * 512:(tb + 1) * 512], ot[:, :], [orr], [], okey)
        tk.finish()
    return nc


class Ctx:
    pass


def ld_const(tk, name, shape, dram_ap, dt=F32, q="sp"):
    t = tk.sb(name, shape, dt)
    r = Res(name)
    tk.dma(q, t, dram_ap, [], [r], "c_" + name)
    return t, r


def emit_L(tk, cx, S):
    fm, yT, pp = cx.fm, cx.yT, cx.pp
    TB = 512
    NT = S // TB
    lp, lpr = ld_const(tk, "lp", [128, 16], cx.lp)
    lw, lwr = ld_const(tk, "lw", [128, 4, 128], cx.lw.rearrange("p (a e) -> p a e", a=4), BF16, "pool")
    cc = tk.sb("l_cc", [128, 4], F32)
    ccr = Res("l_cc")
    tmp = tk.sb("l_tmp", [128, 2], F32)
    tmpr = Res("l_tmp")
    tk.act(lambda e: e.activation(out=tmp[:, :], in_=lp[:, 14:16], func=AF.Exp, scale=-1.0), [lpr], [tmpr])
    tk.act(lambda e: e.activation(out=tmp[:, :], in_=tmp[:, :], func=AF.Ln, bias=cx.one1[:, 0:1]), [tmpr, cx.one1r], [tmpr])
    tk.dve(lambda e: e.tensor_scalar(out=cc[:, 0:2], in0=tmp[:, :], scalar1=-8.0, scalar2=None, op0=ALU.mult), [tmpr], [ccr])
    tk.dve(lambda e: e.tensor_scalar(out=cc[:, 2:4], in0=tmp[:, :], scalar1=-16.0, scalar2=None, op0=ALU.mult), [tmpr], [ccr])
    hl = tk.sb("l_hl", [128, 2], F32)
    hlr = Res("l_hl")
    xin = Buf(tk, "l_xin", [128, TB + 3], F32, 2)
    xc = Buf(tk, "l_xc", [128, TB], F32, 1)
    xcb = Buf(tk, "l_xcb", [128, TB], BF16, 1)
    rr = Buf(tk, "l_r", [128, TB], F32, 1)
    ii = Buf(tk, "l_i", [128, TB], F32, 1)
    aa = Buf(tk, "l_a", [128, TB], F32, 1)
    ss = Buf(tk, "l_s", [128, TB], F32, 1)
    uu = Buf(tk, "l_u", [128, TB], F32, 1)
    hh = Buf(tk, "l_h", [128, TB], F32, 1)
    gg = Buf(tk, "l_g", [128, TB], F32, 1)
    ob = Buf(tk, "l_o", [128, TB], BF16, 1)
    for n in range(2):
        for tb in range(NT):
            xt, xr, xk = xin.next()
            r0 = L_X + n * 128
            if tb == 0:
                tk.dve(lambda e, xt=xt: e.memset(xt[:, 0:3], 0.0), [], [xr])
                tk.dma("sp", xt[:, 3:TB + 3], fm[r0:r0 + 128, 0:TB], [], [xr], xk)
            else:
                tk.dma("sp", xt[:, 0:TB + 3], fm[r0:r0 + 128, tb * TB - 3:(tb + 1) * TB], [], [xr], xk)
            ct, cr, _ = xc.next()
            tk.dve(lambda e, ct=ct, xt=xt, n=n: e.tensor_scalar(out=ct[:, :], in0=xt[:, 0:TB], scalar1=lp[:, n * 4:n * 4 + 1], scalar2=lp[:, 8 + n:9 + n], op0=ALU.mult, op1=ALU.add), [xr, lpr], [cr])
            for k in range(1, 4):
                eng = tk.dve
                eng(lambda e, ct=ct, xt=xt, n=n, k=k: e.scalar_tensor_tensor(out=ct[:, :], in0=xt[:, k:k + TB], scalar=lp[:, n * 4 + k:n * 4 + k + 1], in1=ct[:, :], op0=ALU.mult, op1=ALU.add), [xr, lpr, cr], [cr])
            bt, br, _ = xcb.next()
            tk.act(lambda e, bt=bt, ct=ct: e.activation(out=bt[:, :], in_=ct[:, :], func=AF.Copy), [cr], [br])
            rt, rrr, _ = rr.next()
            it, ir, _ = ii.next()
            pt, pr = pp.next()
            tk.pe(lambda e, pt=pt, bt=bt, n=n: e.matmul(pt[:, 0:TB], lw[:, n, :], bt[:, :], start=True, stop=True), [lwr, br], [pr])
            tk.act(lambda e, rt=rt, pt=pt, n=n: e.activation(out=rt[:, :], in_=pt[:, 0:TB], func=AF.Sigmoid, bias=lp[:, 10 + n:11 + n]), [pr, lpr], [rrr])
            pt2, pr2 = pp.next()
            tk.pe(lambda e, pt2=pt2, bt=bt, n=n: e.matmul(pt2[:, 0:TB], lw[:, 2 + n, :], bt[:, :], start=True, stop=True), [lwr, br], [pr2])
            tk.act(lambda e, it=it, pt2=pt2, n=n: e.activation(out=it[:, :], in_=pt2[:, 0:TB], func=AF.Sigmoid, bias=lp[:, 12 + n:13 + n]), [pr2, lpr], [ir])
            at, ar, _ = aa.next()
            st, sr, _ = ss.next()
            tk.act(lambda e, at=at, rt=rt, n=n: e.activation(out=at[:, :], in_=rt[:, :], func=AF.Exp, scale=cc[:, n:n + 1]), [rrr, ccr], [ar])
            tk.act(lambda e, st=st, rt=rt, n=n: e.activation(out=st[:, :], in_=rt[:, :], func=AF.Exp, scale=cc[:, 2 + n:3 + n]), [rrr, ccr], [sr])
            tk.act(lambda e, st=st: e.activation(out=st[:, :], in_=st[:, :], func=AF.Sqrt, scale=-1.0, bias=cx.one1[:, 0:1]), [sr, cx.one1r], [sr])
            ut, ur, _ = uu.next()
            tk.pool(lambda e, ut=ut, it=it, ct=ct: e.tensor_tensor(out=ut[:, :], in0=it[:, :], in1=ct[:, :], op=ALU.mult), [ir, cr], [ur])
            tk.dve(lambda e, ut=ut, st=st: e.tensor_tensor(out=ut[:, :], in0=ut[:, :], in1=st[:, :], op=ALU.mult), [ur, sr], [ur])
            ht, hr, _ = hh.next()
            if tb == 0:
                tk.dve(lambda e, ht=ht, at=at, ut=ut: e.tensor_tensor_scan(out=ht[:, :], data0=at[:, :], data1=ut[:, :], initial=0.0, op0=ALU.mult, op1=ALU.add), [ar, ur], [hr])
            else:
                tk.dve(lambda e, at=at, ut=ut, n=n: e.scalar_tensor_tensor(out=ut[:, 0:1], in0=at[:, 0:1], scalar=hl[:, n:n + 1], in1=ut[:, 0:1], op0=ALU.mult, op1=ALU.add), [ar, ur, hlr], [ur])
                tk.dve(lambda e, ht=ht, at=at, ut=ut: e.tensor_tensor_scan(out=ht[:, :], data0=at[:, :], data1=ut[:, :], initial=0.0, op0=ALU.mult, op1=ALU.add), [ar, ur], [hr])
            tk.dve(lambda e, ht=ht, n=n: e.tensor_copy(out=hl[:, n:n + 1], in_=ht[:, TB - 1:TB]), [hr], [hlr])
            gt, gr, gk = gg.next()
            g0 = L_G + n * 128
            tk.dma("sp", gt[:, :], fm[g0:g0 + 128, tb * TB:(tb + 1) * TB], [], [gr], gk)
            tk.act(lambda e, gt=gt: e.activation(out=gt[:, :], in_=gt[:, :], func=AF.Silu), [gr], [gr])
            ot, orr, ok = ob.next()
            tk.pool(lambda e, ot=ot, ht=ht, gt=gt: e.tensor_tensor(out=ot[:, :], in0=ht[:, :], in1=gt[:, :], op=ALU.mult), [hr, gr], [orr])
            y0 = 768 + n * 128
            tk.dma("sp", yT[y0:y0 + 128, tb * TB:(tb + 1) * TB], ot[:, :], [orr], [], ok)


def emit_R(tk, cx, S):
    fm, tm, yT, pp = cx.fm, cx.tm, cx.yT, cx.pp
    C = 256
    NCH = S // C
    rt_, rtr = ld_const(tk, "rt", [128, 773], cx.rt)
    dmat = lambda jt: rt_[:, jt * 256:(jt + 1) * 256]
    qdec = rt_[:, 512:768]
    Sf = tk.sb("r_Sf", [128, 2, 256], F32)
    Sfr = Res("r_Sf")
    Sb = tk.sb("r_Sb", [128, 2, 256], BF16)
    Sbr = Res("r_Sb")
    qf = Buf(tk, "r_qf", [128, 2, 256], F32, 2)
    kf = Buf(tk, "r_kf", [128, 2, 256], F32, 2)
    gf = Buf(tk, "r_gf", [128, 2, 256], F32, 2)
    vf = Buf(tk, "r_vf", [128, 2, 256], F32, 2)
    qb = Buf(tk, "r_qb", [128, 2, 256], BF16, 2)
    qdb = Buf(tk, "r_qdb", [128, 2, 256], BF16, 2)
    kb = Buf(tk, "r_kb", [128, 2, 256], BF16, 2)
    vb = Buf(tk, "r_vb", [128, 2, 256], BF16, 2)
    ktm = Buf(tk, "r_ktm", [128, 2, 256], BF16, 2)
    inb = Buf(tk, "r_inb", [128, 2, 256], BF16, 2)
    sq = Buf(tk, "r_sq", [128, 2, 256], F32, 2)
    rs = Buf(tk, "r_rs", [128, 256], F32, 2)
    tt = Buf(tk, "r_t", [128, 2, 256], F32, 2)
    ob = Buf(tk, "r_o", [128, 2, 256], BF16, 2)
    for c in range(NCH):
        cs = slice(c * C, (c + 1) * C)
        qt, qr, qk = qf.next()
        tk.dma("sp", qt, fm[R_Q:R_Q + 256, cs].rearrange("(t p) s -> p t s", p=128), [], [qr], qk)
        kt_, kr, kk = kf.next()
        tk.dma("sp", kt_, fm[R_K:R_K + 256, cs].rearrange("(t p) s -> p t s", p=128), [], [kr], kk)
        gt, gr, gk = gf.next()
        tk.dma("sp", gt, fm[R_G:R_G + 256, cs].rearrange("(t p) s -> p t s", p=128), [], [gr], gk)
        vt_, vr, vk = vf.next()
        tk.dma("sp", vt_, tm[cs, 0:256].rearrange("(t p) v -> p t v", p=128), [], [vr], vk)
        qbt, qbr, _ = qb.next()
        tk.act(lambda e, qbt=qbt, qt=qt: e.activation(out=qbt, in_=qt, func=AF.Copy), [qr], [qbr])
        qdt, qdr, _ = qdb.next()
        for d_ in range(2):
            tk.dve(lambda e, qdt=qdt, qt=qt, d_=d_: e.tensor_tensor(out=qdt[:, d_, :], in0=qt[:, d_, :], in1=qdec, op=ALU.mult), [qr, rtr], [qdr])
        kbt, kbr, _ = kb.next()
        tk.act(lambda e, kbt=kbt, kt_=kt_: e.activation(out=kbt, in_=kt_, func=AF.Copy, scale=0.0625), [kr], [kbr])
        vbt, vbr, _ = vb.next()
        tk.pool(lambda e, vbt=vbt, vt_=vt_: e.tensor_copy(out=vbt, in_=vt_), [vr], [vbr])
        ktt, ktr, _ = ktm.next()
        if c < NCH - 1:
            for jt in range(2):
                pt, pr = pp.next()
                for d_ in range(2):
                    tk.pe(lambda e, pt=pt, kbt=kbt, d_=d_, jt=jt: e.matmul(pt[:, d_ * 128:(d_ + 1) * 128], kbt[:, d_, jt * 128:(jt + 1) * 128], cx.identb[:, :], start=True, stop=True), [kbr, cx.identbr], [pr])
                tk.dve(lambda e, ktt=ktt, pt=pt, jt=jt: e.tensor_scalar(out=ktt[:, jt, :], in0=pt[:, 0:256], scalar1=rt_[:, 768 + jt:769 + jt], scalar2=None, op0=ALU.mult), [pr, rtr], [ktr])
        it_, ir, _ = inb.next()
        for jt in range(2):
            pt, pr = pp.next()
            for d_ in range(2):
                tk.pe(lambda e, pt=pt, kbt=kbt, qbt=qbt, d_=d_, jt=jt: e.matmul(pt[:, 0:256], kbt[:, d_, jt * 128:(jt + 1) * 128], qbt[:, d_, :], start=(d_ == 0), stop=(d_ == 1)), [kbr, qbr], [pr], sig=(d_ == 1))
            tk.dve(lambda e, it_=it_, pt=pt, jt=jt: e.tensor_tensor(out=it_[:, jt, :], in0=pt[:, 0:256], in1=dmat(jt), op=ALU.mult), [pr, rtr], [ir])
        pys = []
        for vt2 in range(2):
            pt, pr = pp.next()
            pys.append((pt, pr))
            nmm = 2 + (2 if c > 0 else 0)
            i_ = 0
            for jt in range(2):
                tk.pe(lambda e, pt=pt, vbt=vbt, it_=it_, jt=jt, vt2=vt2, i_=i_, nmm=nmm: e.matmul(pt[:, 0:256], vbt[:, jt, vt2 * 128:(vt2 + 1) * 128], it_[:, jt, :], start=(i_ == 0), stop=(i_ == nmm - 1)), [vbr, ir], [pr], sig=(i_ == nmm - 1))
                i_ += 1
            if c > 0:
                for d_ in range(2):
                    tk.pe(lambda e, pt=pt, qdt=qdt, d_=d_, vt2=vt2, i_=i_, nmm=nmm: e.matmul(pt[:, 0:256], Sb[:, d_, vt2 * 128:(vt2 + 1) * 128], qdt[:, d_, :], start=False, stop=(i_ == nmm - 1)), [Sbr, qdr], [pr], sig=(i_ == nmm - 1))
                    i_ += 1
        sqt, sqr, _ = sq.next()
        for vt2 in range(2):
            pt, pr = pys[vt2]
            tk.act(lambda e, sqt=sqt, pt=pt, vt2=vt2: e.activation(out=sqt[:, vt2, :], in_=pt[:, 0:256], func=AF.Square), [pr], [sqr])
        pn, pnr = pp.next()
        for vt2 in range(2):
            tk.pe(lambda e, pn=pn, sqt=sqt, vt2=vt2: e.matmul(pn[:, 0:256], cx.onesf[:, :], sqt[:, vt2, :], start=(vt2 == 0), stop=(vt2 == 1)), [cx.onesfr, sqr], [pnr], sig=(vt2 == 1))
        rst, rsr, _ = rs.next()
        tk.act(lambda e, rst=rst, pn=pn: e.activation(out=rst, in_=pn[:, 0:256], func=AF.Sqrt, scale=1.0 / 256, bias=cx.eps1[:, 0:1]), [pnr, cx.eps1r], [rsr])
        tk.dve(lambda e, rst=rst: e.reciprocal(out=rst, in_=rst), [rsr], [rsr])
        tk.act(lambda e, gt=gt: e.activation(out=gt, in_=gt, func=AF.Silu), [gr], [gr])
        t_, tr, _ = tt.next()
        ot, orr, ok = ob.next()
        for vt2 in range(2):
            pt, pr = pys[vt2]
            tk.dve(lambda e, t_=t_, pt=pt, rst=rst, vt2=vt2: e.scalar_tensor_tensor(out=t_[:, vt2, :], in0=pt[:, 0:256], scalar=rt_[:, 771 + vt2:772 + vt2], in1=rst, op0=ALU.mult, op1=ALU.mult), [pr, rsr, rtr], [tr])
        tk.pool(lambda e, ot=ot, t_=t_, gt=gt: e.tensor_tensor(out=ot, in0=t_, in1=gt, op=ALU.mult), [tr, gr], [orr])
        tk.dma("sp", yT[0:256, cs].rearrange("(t p) s -> p t s", p=128), ot, [orr], [], ok)
        if c < NCH - 1:
            for d_ in range(2):
                pt, pr = pp.next()
                for jt in range(2):
                    tk.pe(lambda e, pt=pt, ktt=ktt, vbt=vbt, d_=d_, jt=jt: e.matmul(pt[:, 0:256], ktt[:, jt, d_ * 128:(d_ + 1) * 128], vbt[:, jt, :], start=(jt == 0), stop=(jt == 1)), [ktr, vbr], [pr], sig=(jt == 1))
                if c == 0:
                    tk.dve(lambda e, pt=pt, d_=d_: e.tensor_copy(out=Sf[:, d_, :], in_=pt[:, 0:256]), [pr], [Sfr])
                else:
                    tk.dve(lambda e, pt=pt, d_=d_: e.scalar_tensor_tensor(out=Sf[:, d_, :], in0=Sf[:, d_, :], scalar=rt_[:, 770:771], in1=pt[:, 0:256], op0=ALU.mult, op1=ALU.add), [pr, Sfr, rtr], [Sfr])
            tk.act(lambda e: e.activation(out=Sb, in_=Sf, func=AF.Copy), [Sfr], [Sbr])


def emit_S(tk, cx, S):
    fm, yT, pp = cx.fm, cx.yT, cx.pp
    C = 256
    NCH = S // C
    st_, str_ = ld_const(tk, "st", [128, 32], cx.st)
    tri = cx.tri
    trir = cx.trir
    abc = tk.sb("s_abc", [128, 4], F32)
    abcr = Res("s_abc")
    tk.act(lambda e: e.activation(out=abc[:, :], in_=st_[:, 28:32], func=AF.Exp), [str_], [abcr])
    tk.dve(lambda e: e.tensor_scalar(out=abc[:, :], in0=abc[:, :], scalar1=-1.0, scalar2=None, op0=ALU.mult), [abcr], [abcr])
    hf = tk.sb("s_hf", [128, 256], F32)
    hfr = Res("s_hf")
    hb = tk.sb("s_hb", [128, 256], BF16)
    hbr = Res("s_hb")
    xin = Buf(tk, "s_xin", [128, 4, C + 3], F32, 1)
    xc = Buf(tk, "s_xc", [128, 4, C], F32, 1)
    xab = Buf(tk, "s_xab", [128, 4, C], BF16, 2)
    zf = Buf(tk, "s_zf", [128, 2, C], F32, 2)
    dtr = Buf(tk, "s_dtr", [4, C], F32, 2)
    dtm = Buf(tk, "s_dtm", [128, 2, 4], F32, 2)
    dam = Buf(tk, "s_dam", [128, 2, 4], F32, 2)
    cstm = Buf(tk, "s_cstm", [128, 2, 4], F32, 2)
    darep = Buf(tk, "s_darep", [128, 2, 128], F32, 2)
    csbc = Buf(tk, "s_csbc", [128, 4, C], F32, 1)
    ecs = Buf(tk, "s_ecs", [128, 4, C], F32, 1)
    cbm = Buf(tk, "s_cbm", [128, 2, C], F32, 2)
    xtm = Buf(tk, "s_xtm", [128, 2, 256], BF16, 2)
    btm = Buf(tk, "s_btm", [128, 2, 128], BF16, 2)
    seg = Buf(tk, "s_seg", [128, C], F32, 3)
    wT = Buf(tk, "s_wT", [128, 4, 2, C], BF16, 1)
    csb = Buf(tk, "s_csb", [128, 4, C], BF16, 1)
    y2 = Buf(tk, "s_y2", [128, 2, C], F32, 2)
    sq = Buf(tk, "s_sq", [128, 2, C], F32, 2)
    rs = Buf(tk, "s_rs", [128, C], F32, 2)
    ob = Buf(tk, "s_o", [128, 2, C], BF16, 2)
    sd = Buf(tk, "s_sd", [128, 2, 4], F32, 2)
    csl = Buf(tk, "s_csl", [128, 4], F32, 2)
    xw = Buf(tk, "s_xw", [128, 2, 256], BF16, 2)
    for c in range(NCH):
        cs = slice(c * C, (c + 1) * C)
        last = (c == NCH - 1)
        xt, xr, xk = xin.next()
        if c == 0:
            tk.dve(lambda e, xt=xt: e.memset(xt[:, :, 0:3], 0.0), [], [xr])
            tk.dma("sp", xt[:, :, 3:C + 3], fm[S_X:S_X + 512, 0:C].rearrange("(t p) s -> p t s", p=128), [], [xr], xk)
        else:
            tk.dma("sp", xt[:, :, 0:C + 3], fm[S_X:S_X + 512, c * C - 3:(c + 1) * C].rearrange("(t p) s -> p t s", p=128), [], [xr], xk)
        ct, cr, _ = xc.next()
        for t4 in range(4):
            eng = tk.dve
            eng(lambda e, ct=ct, xt=xt, t4=t4: e.tensor_scalar(out=ct[:, t4, :], in0=xt[:, t4, 0:C], scalar1=st_[:, t4 * 4:t4 * 4 + 1], scalar2=st_[:, 16 + t4:17 + t4], op0=ALU.mult, op1=ALU.add), [xr, str_], [cr])
            for k in range(1, 4):
                eng(lambda e, ct=ct, xt=xt, t4=t4, k=k: e.scalar_tensor_tensor(out=ct[:, t4, :], in0=xt[:, t4, k:k + C], scalar=st_[:, t4 * 4 + k:t4 * 4 + k + 1], in1=ct[:, t4, :], op0=ALU.mult, op1=ALU.add), [xr, str_, cr], [cr])
        tk.act(lambda e, ct=ct: e.activation(out=ct, in_=ct, func=AF.Silu), [cr], [cr])
        xbt, xbr, _ = xab.next()
        tk.pool(lambda e, xbt=xbt, ct=ct: e.tensor_copy(out=xbt, in_=ct), [cr], [xbr])
        zt, zr, zk = zf.next()
        tk.dma("sp", zt, fm[S_Z:S_Z + 256, cs].rearrange("(t p) s -> p t s", p=128), [], [zr], zk)
        dt_, dr, dk = dtr.next()
        tk.dma("sp", dt_, fm[S_DT:S_DT + 4, cs], [], [dr], dk)
        pt, pr = pp.next()
        for jt in range(2):
            tk.pe(lambda e, pt=pt, dt_=dt_, jt=jt: e.matmul(pt[:, jt * 4:(jt + 1) * 4], dt_[0:4, jt * 128:(jt + 1) * 128], cx.identf[0:4, 0:4], start=True, stop=True), [dr, cx.identfr], [pr])
        dmt, dmr, _ = dtm.next()
        for jt in range(2):
            tk.dve(lambda e, dmt=dmt, pt=pt, jt=jt: e.tensor_tensor(out=dmt[:, jt, :], in0=pt[:, jt * 4:(jt + 1) * 4], in1=st_[:, 24:28], op=ALU.add), [pr, str_], [dmr])
        tk.act(lambda e, dmt=dmt: e.activation(out=dmt, in_=dmt, func=AF.Exp), [dmr], [dmr])
        tk.act(lambda e, dmt=dmt: e.activation(out=dmt, in_=dmt, func=AF.Ln, bias=cx.one1[:, 0:1]), [dmr, cx.one1r], [dmr])
        dat, dar, _ = dam.next()
        for jt in range(2):
            tk.dve(lambda e, dat=dat, dmt=dmt, jt=jt: e.tensor_tensor(out=dat[:, jt, :], in0=dmt[:, jt, :], in1=abc[:, :], op=ALU.mult), [dmr, abcr], [dar])
        pc, pcr = pp.next()
        tk.pe(lambda e, pc=pc, dat=dat: e.matmul(pc[:, 0:4], tri[:, 0, 0:128], dat[:, 0, :], start=True, stop=True), [trir, dar], [pcr])
        tk.pe(lambda e, pc=pc, dat=dat: e.matmul(pc[:, 4:8], tri[:, 0, 128:256], dat[:, 0, :], start=True, stop=False), [trir, dar], [pcr], sig=False)
        tk.pe(lambda e, pc=pc, dat=dat: e.matmul(pc[:, 4:8], tri[:, 1, 128:256], dat[:, 1, :], start=False, stop=True), [trir, dar], [pcr])
        cst, csr, _ = cstm.next()
        tk.act(lambda e, cst=cst, pc=pc: e.activation(out=cst, in_=pc[:, 0:8].rearrange("p (a b) -> p a b", a=2), func=AF.Copy), [pcr], [csr])
        cbt, cbr, _ = csbc.next()
        ect, ecr, _ = ecs.next()
        for r in range(4):
            drt, drr, _ = darep.next()
            for jt in range(2):
                tk.dve(lambda e, drt=drt, dat=dat, jt=jt, r=r: e.tensor_scalar(out=drt[:, jt, :], in0=cx.onesf[:, :], scalar1=dat[:, jt, r:r + 1], scalar2=None, op0=ALU.mult), [dar, cx.onesfr], [drr])
            pb, pbr = pp.next()
            for jt in range(2):
                tk.pe(lambda e, pb=pb, drt=drt, jt=jt: e.matmul(pb[:, 0:C], drt[:, jt, :], tri[:, jt, :], start=(jt == 0), stop=(jt == 1)), [drr, trir], [pbr], sig=(jt == 1))
            tk.act(lambda e, cbt=cbt, pb=pb, r=r: e.activation(out=cbt[:, r, :], in_=pb[:, 0:C], func=AF.Copy), [pbr], [cbr])
            tk.act(lambda e, ect=ect, pb=pb, r=r: e.activation(out=ect[:, r, :], in_=pb[:, 0:C], func=AF.Exp), [pbr], [ecr])
        cmt, cmr, _ = cbm.next()
        for jt in range(2):
            pq, pqr = pp.next()
            tk.pe(lambda e, pq=pq, xbt=xbt, jt=jt: e.matmul(pq[:, 0:C], xbt[:, 2, jt * 128:(jt + 1) * 128], xbt[:, 3, :], start=True, stop=True), [xbr], [pqr])
            tk.dve(lambda e, cmt=cmt, pq=pq, jt=jt: e.tensor_tensor(out=cmt[:, jt, :], in0=pq[:, 0:C], in1=tri[:, jt, :], op=ALU.mult), [pqr, trir], [cmr])
        xtt, xtr, _ = xtm.next()
        btt, btr, _ = btm.next()
        for jt in range(2):
            px, pxr = pp.next()
            for t2 in range(2):
                tk.pe(lambda e, px=px, xbt=xbt, jt=jt, t2=t2: e.matmul(px[:, t2 * 128:(t2 + 1) * 128], xbt[:, t2, jt * 128:(jt + 1) * 128], cx.identb[:, :], start=True, stop=True), [xbr, cx.identbr], [pxr])
            tk.pe(lambda e, px=px, xbt=xbt, jt=jt: e.matmul(px[:, 256:384], xbt[:, 2, jt * 128:(jt + 1) * 128], cx.identb[:, :], start=True, stop=True), [xbr, cx.identbr], [pxr])
            tk.act(lambda e, xtt=xtt, px=px, jt=jt: e.activation(out=xtt[:, jt, :], in_=px[:, 0:256], func=AF.Copy), [pxr], [xtr])
            tk.act(lambda e, btt=btt, px=px, jt=jt: e.activation(out=btt[:, jt, :], in_=px[:, 256:384], func=AF.Copy), [pxr], [btr])
        wt, wr, _ = wT.next()
        cbb, cbbr, _ = csb.next()
        for r in range(4):
            for jt in range(2):
                sg, sgr, _ = seg.next()
                tk.dve(lambda e, sg=sg, cbt=cbt, cst=cst, r=r, jt=jt: e.tensor_scalar(out=sg, in0=cbt[:, r, :], scalar1=cst[:, jt, r:r + 1], scalar2=0.0, op0=ALU.subtract, op1=ALU.min), [cbr, csr], [sgr])
                tk.act(lambda e, sg=sg: e.activation(out=sg, in_=sg, func=AF.Exp), [sgr], [sgr])
                tk.dve(lambda e, wt=wt, sg=sg, dmt=dmt, cmt=cmt, r=r, jt=jt: e.scalar_tensor_tensor(out=wt[:, r, jt, :], in0=sg, scalar=dmt[:, jt, r:r + 1], in1=cmt[:, jt, :], op0=ALU.mult, op1=ALU.mult), [sgr, dmr, cmr], [wr])
            if c > 0:
                tk.pool(lambda e, cbb=cbb, ct=ct, ect=ect, r=r: e.tensor_tensor(out=cbb[:, r, :], in0=ct[:, 3, :], in1=ect[:, r, :], op=ALU.mult), [cr, ecr], [cbbr])
        pys = []
        for t2 in range(2):
            py, pyr = pp.next()
            pys.append((py, pyr))
            for hh in range(2):
                r = 2 * t2 + hh
                n_ = 3 if c > 0 else 2
                for jt in range(2):
                    tk.pe(lambda e, py=py, xtt=xtt, wt=wt, r=r, hh=hh, jt=jt, n_=n_: e.matmul(py[hh * 64:(hh + 1) * 64, 0:C], xtt[:, jt, r * 64:(r + 1) * 64], wt[:, r, jt, :], start=(jt == 0), stop=(jt == n_ - 1)), [xtr, wr], [pyr], sig=(jt == n_ - 1))
                if c > 0:
                    tk.pe(lambda e, py=py, cbb=cbb, r=r, hh=hh: e.matmul(py[hh * 64:(hh + 1) * 64, 0:C], hb[:, r * 64:(r + 1) * 64], cbb[:, r, :], start=False, stop=True), [hbr, cbbr], [pyr])
        if not last:
            clt, clr, _ = csl.next()
            for r in range(4):
                tk.dve(lambda e, clt=clt, cbt=cbt, r=r: e.tensor_copy(out=clt[:, r:r + 1], in_=cbt[:, r, C - 1:C]), [cbr], [clr])
            sdt, sdr, _ = sd.next()
            for jt in range(2):
                tk.dve(lambda e, sdt=sdt, clt=clt, cst=cst, jt=jt: e.tensor_tensor(out=sdt[:, jt, :], in0=clt[:, :], in1=cst[:, jt, :], op=ALU.subtract), [clr, csr], [sdr])
            tk.act(lambda e, sdt=sdt: e.activation(out=sdt, in_=sdt, func=AF.Exp), [sdr], [sdr])
            tk.dve(lambda e, sdt=sdt, dmt=dmt: e.tensor_tensor(out=sdt, in0=sdt, in1=dmt, op=ALU.mult), [sdr, dmr], [sdr])
            xwt, xwr, _ = xw.next()
            for jt in range(2):
                for r in range(4):
                    eng = tk.dve if r % 2 == 0 else tk.pool
                    eng(lambda e, xwt=xwt, xtt=xtt, sdt=sdt, jt=jt, r=r: e.tensor_scalar(out=xwt[:, jt, r * 64:(r + 1) * 64], in0=xtt[:, jt, r * 64:(r + 1) * 64], scalar1=sdt[:, jt, r:r + 1], scalar2=None, op0=ALU.mult), [xtr, sdr], [xwr])
            pst, pstr = pp.next()
            for jt in range(2):
                tk.pe(lambda e, pst=pst, btt=btt, xwt=xwt, jt=jt: e.matmul(pst[:, 0:256], btt[:, jt, :], xwt[:, jt, :], start=(jt == 0), stop=(jt == 1)), [btr, xwr], [pstr], sig=(jt == 1))
            if c == 0:
                tk.dve(lambda e, pst=pst: e.tensor_copy(out=hf[:, :], in_=pst[:, 0:256]), [pstr], [hfr])
            else:
                for r in range(4):
                    tk.dve(lambda e, pst=pst, ect=ect, r=r: e.scalar_tensor_tensor(out=hf[:, r * 64:(r + 1) * 64], in0=hf[:, r * 64:(r + 1) * 64], scalar=ect[:, r, C - 1:C], in1=pst[:, r * 64:(r + 1) * 64], op0=ALU.mult, op1=ALU.add), [pstr, ecr, hfr], [hfr])
            tk.act(lambda e: e.activation(out=hb[:, :], in_=hf[:, :], func=AF.Copy), [hfr], [hbr])
        tk.act(lambda e, zt=zt: e.activation(out=zt, in_=zt, func=AF.Silu), [zr], [zr])
        y2t, y2r, _ = y2.next()
        sqt, sqr, _ = sq.next()
        for t2 in range(2):
            py, pyr = pys[t2]
            tk.dve(lambda e, y2t=y2t, ct=ct, py=py, t2=t2: e.scalar_tensor_tensor(out=y2t[:, t2, :], in0=ct[:, t2, :], scalar=st_[:, 20 + t2:21 + t2], in1=py[:, 0:C], op0=ALU.mult, op1=ALU.add), [cr, str_, pyr], [y2r])
        tk.pool(lambda e, y2t=y2t, zt=zt: e.tensor_tensor(out=y2t, in0=y2t, in1=zt, op=ALU.mult), [y2r, zr], [y2r])
        tk.act(lambda e, sqt=sqt, y2t=y2t: e.activation(out=sqt, in_=y2t, func=AF.Square), [y2r], [sqr])
        pn, pnr = pp.next()
        for t2 in range(2):
            tk.pe(lambda e, pn=pn, sqt=sqt, t2=t2: e.matmul(pn[:, 0:C], cx.onesf[:, :], sqt[:, t2, :], start=(t2 == 0), stop=(t2 == 1)), [cx.onesfr, sqr], [pnr], sig=(t2 == 1))
        rst, rsr, _ = rs.next()
        tk.act(lambda e, rst=rst, pn=pn: e.activation(out=rst, in_=pn[:, 0:C], func=AF.Sqrt, scale=1.0 / 256, bias=cx.eps1[:, 0:1]), [pnr, cx.eps1r], [rsr])
        tk.dve(lambda e, rst=rst: e.reciprocal(out=rst, in_=rst), [rsr], [rsr])
        ot, orr, ok = ob.next()
        for t2 in range(2):
            tk.dve(lambda e, ot=ot, y2t=y2t, rst=rst, t2=t2: e.scalar_tensor_tensor(out=ot[:, t2, :], in0=y2t[:, t2, :], scalar=st_[:, 22 + t2:23 + t2], in1=rst, op0=ALU.mult, op1=ALU.mult), [y2r, rsr, str_], [orr])
        tk.dma("sp", yT[512:768, cs].rearrange("(t p) s -> p t s", p=128), ot, [orr], [], ok)


def emit_M(tk, cx, S):
    fm, tm, yT, pp = cx.fm, cx.tm, cx.yT, cx.pp
    L = 256
    NB = S // L
    NTC = S // 512
    mt_, mtr = ld_const(tk, "mt", [128, 4], cx.mt)
    bdc, bdcr = ld_const(tk, "mbdc", [16, 32], cx.mbdc)
    oh, ohr = ld_const(tk, "moh", [17, 17, 128], cx.moh.rearrange("p (a b) -> p a b", a=17), BF16, "pool")
    cb_, cbr_ = ld_const(tk, "mcb", [128, 2, 256], cx.mcb.rearrange("p (a b) -> p a b", a=2), BF16, "pool")
    pm, pmr = ld_const(tk, "mpm", [128, 16, 16], cx.mpm.rearrange("p (a b) -> p a b", a=16))
    qb = tk.sb("m_qb", [128, S], BF16)
    qbr = Res("m_qb")
    kb = tk.sb("m_kb", [128, S], BF16)
    kbr = Res("m_kb")
    vb = tk.sb("m_vb", [128, S // 128, 128], BF16)
    vbr = Res("m_vb")
    br_ = tk.sb("m_br", [17, S], BF16)
    brr = Res("m_br")
    kmT = tk.sb("m_kmT", [128, 16], F32)
    kmr = Res("m_kmT")
    qf = Buf(tk, "m_qf", [128, 512], F32, 2)
    kf = Buf(tk, "m_kf", [128, 512], F32, 2)
    vf = Buf(tk, "m_vf", [128, 4, 128], F32, 2)
    gm = Buf(tk, "m_gm", [128, 16], F32, 2)
    t8 = Buf(tk, "m_t8", [128, 8], F32, 2)
    sel = Buf(tk, "m_sel", [128, 16], F32, 2)
    pT = Buf(tk, "m_pT", [128, 256], BF16, 4)
    gf = Buf(tk, "m_gf", [128, 256], F32, 2)
    rd = Buf(tk, "m_rd", [128, 256], F32, 2)
    o1 = Buf(tk, "m_o1", [128, 256], F32, 2)
    ob = Buf(tk, "m_o", [128, 256], BF16, 2)
    scale = 128.0 ** -0.5
    for hh in range(2):
        tk.dve(lambda e: e.memset(kmT[:, :], 0.0), [], [kmr])
        tk.pool(lambda e: e.memset(br_[:, :], 0.0), [], [brr])
        tk.dma("pool", br_[16:17, :], cx.mli[hh:hh + 1, :], [], [brr], "m_br")
        for tc in range(NTC):
            ts_ = slice(tc * 512, (tc + 1) * 512)
            qt, qr, qk = qf.next()
            tk.dma("sp", qt, fm[M_Q + hh * 128:M_Q + (hh + 1) * 128, ts_], [], [qr], qk)
            kt_, kr, kk = kf.next()
            tk.dma("sp", kt_, fm[M_K + hh * 128:M_K + (hh + 1) * 128, ts_], [], [kr], kk)
            vt_, vr, vk = vf.next()
            tk.dma("sp", vt_, tm[ts_, 256 + hh * 128:256 + (hh + 1) * 128].rearrange("(t p) v -> p t v", p=128), [], [vr], vk)
            tk.act(lambda e, qt=qt, ts_=ts_: e.activation(out=qb[:, ts_], in_=qt, func=AF.Copy, scale=scale), [qr], [qbr])
            tk.pool(lambda e, kt_=kt_, ts_=ts_: e.tensor_copy(out=kb[:, ts_], in_=kt_), [kr], [kbr])
            tk.dve(lambda e, kt_=kt_, tc=tc: e.tensor_reduce(out=kmT[:, tc * 2:tc * 2 + 2], in_=kt_.rearrange("p (a b) -> p a b", a=2), axis=AX.X, op=ALU.add), [kr], [kmr])
            tk.dve(lambda e, vt_=vt_, tc=tc: e.tensor_copy(out=vb[:, tc * 4:(tc + 1) * 4, :], in_=vt_), [vr], [vbr])
            for tt in range(4):
                t = tc * 4 + tt
                a = t // 2
                if a == 0:
                    continue
                pg, pgr = pp.next()
                tk.pe(lambda e, pg=pg, qt=qt, tt=tt: e.matmul(pg[:, 0:16], qt[:, tt * 128:(tt + 1) * 128], kmT[:, :], start=True, stop=True), [qr, kmr], [pgr])
                gmt, gmr, _ = gm.next()
                tk.dve(lambda e, gmt=gmt, pg=pg, a=a: e.tensor_tensor(out=gmt, in0=pg[:, 0:16], in1=pm[:, a, :], op=ALU.add), [pgr, pmr], [gmr])
                t8t, t8r, _ = t8.next()
                tk.dve(lambda e, t8t=t8t, gmt=gmt: e.max(out=t8t, in_=gmt), [gmr], [t8r])
                st, sr, _ = sel.next()
                tk.dve(lambda e, st=st, gmt=gmt, t8t=t8t: e.tensor_scalar(out=st, in0=gmt, scalar1=t8t[:, 2:3], scalar2=None, op0=ALU.is_ge), [gmr, t8r], [sr])
                tk.dve(lambda e, st=st: e.tensor_scalar(out=st, in0=st, scalar1=-1.0, scalar2=-NEG, op0=ALU.add, op1=ALU.mult), [sr], [sr])
                tk.dve(lambda e, st=st, a=a: e.tensor_tensor(out=st, in0=st, in1=pm[:, a, :], op=ALU.min), [sr, pmr], [sr])
                pt_, ptr = pp.next()
                tk.pe(lambda e, pt_=pt_, st=st: e.matmul(pt_[0:16, 0:128], st[:, :], cx.identf[:, :], start=True, stop=True), [sr, cx.identfr], [ptr])
                tk.dve(lambda e, pt_=pt_, t=t, a=a, hh=hh: e.tensor_scalar(out=br_[0:16, t * 128:(t + 1) * 128], in0=pt_[0:16, 0:128], scalar1=bdc[0:16, hh * 16 + a:hh * 16 + a + 1], scalar2=None, op0=ALU.add), [ptr, bdcr], [brr])
        for a in range(NB):
            as_ = slice(a * L, (a + 1) * L)
            po, por = cx.mpo[a % 2]
            pd, pdr = cx.mpd[a % 2]
            ntile = (a + 1) * 2
            i_ = 0
            for n in range(a + 1):
                for jt in range(2):
                    ps_, psr = pp.next()
                    diag = (n == a)
                    tk.pe(lambda e, ps_=ps_, n=n, jt=jt, as_=as_: e.matmul(ps_[:, 0:L], kb[:, n * L + jt * 128:n * L + (jt + 1) * 128], qb[:, as_], start=True, stop=False), [kbr, qbr], [psr], sig=False)
                    tk.pe(lambda e, ps_=ps_, n=n, diag=diag, as_=as_: e.matmul(ps_[:, 0:L], oh[0:17, (16 if diag else n), :], br_[0:17, as_], start=False, stop=(not diag)), [ohr, brr], [psr], sig=(not diag))
                    if diag:
                        tk.pe(lambda e, ps_=ps_, jt=jt: e.matmul(ps_[:, 0:L], cx.identb[:, :], cb_[:, jt, :], start=False, stop=True), [cx.identbr, cbr_], [psr])
                    ptt, ptr2, _ = pT.next()
                    tk.act(lambda e, ptt=ptt, ps_=ps_, jt=jt, hh=hh: e.activation(out=ptt, in_=ps_[:, 0:L], func=AF.Exp, bias=mt_[:, hh * 2 + jt:hh * 2 + jt + 1]), [psr, mtr], [ptr2])
                    tk.pe(lambda e, po=po, ptt=ptt, n=n, jt=jt, i_=i_, ntile=ntile: e.matmul(po[:, 0:L], vb[:, n * 2 + jt, :], ptt, start=(i_ == 0), stop=(i_ == ntile - 1)), [vbr, ptr2], [por], sig=False)
                    tk.pe(lambda e, pd=pd, ptt=ptt, i_=i_, ntile=ntile: e.matmul(pd[:, 0:L], cx.onesb[:, :], ptt, start=(i_ == 0), stop=(i_ == ntile - 1)), [cx.onesbr, ptr2], [pdr, por])
                    i_ += 1
            rdt, rdr, _ = rd.next()
            tk.dve(lambda e, rdt=rdt, pd=pd: e.reciprocal(out=rdt, in_=pd[:, 0:L]), [pdr], [rdr])
            gt, gr, gk = gf.next()
            tk.dma("sp", gt, fm[M_G + hh * 128:M_G + (hh + 1) * 128, as_], [], [gr], gk)
            tk.act(lambda e, gt=gt: e.activation(out=gt, in_=gt, func=AF.Silu), [gr], [gr])
            o1t, o1r, _ = o1.next()
            tk.dve(lambda e, o1t=o1t, po=po, rdt=rdt: e.tensor_tensor(out=o1t, in0=po[:, 0:L], in1=rdt, op=ALU.mult), [por, rdr], [o1r])
            ot, orr, ok = ob.next()
            tk.pool(lambda e, ot=ot, o1t=o1t, gt=gt: e.tensor_tensor(out=ot, in0=o1t, in1=gt, op=ALU.mult), [o1r, gr], [orr])
            tk.dma("sp", yT[256 + hh * 128:256 + (hh + 1) * 128, as_], ot, [orr], [], ok)


X_INPUTS = [("lp", [128, 16]), ("lw", [128, 512]), ("rt", [128, 773]), ("st", [128, 32]), ("cst", [128, 640]),
            ("mt", [128, 4]), ("mbdc", [16, 32]), ("moh", [17, 17 * 128]), ("mcb", [128, 512]), ("mpm", [128, 256])]


def build_X(S=SEQ, which="LRSM"):
    nc = new_nc()
    fm = nc.dram_tensor("fm", [NFM, S], F32, kind="ExternalInput").ap()
    tm = nc.dram_tensor("tm", [S, NTM], F32, kind="ExternalInput").ap()
    yT = nc.dram_tensor("yT", [1024, S], BF16, kind="ExternalOutput").ap()
    cx = Ctx()
    cx.fm, cx.tm, cx.yT = fm, tm, yT
    for name, shape in X_INPUTS:
        setattr(cx, name, nc.dram_tensor(name, shape, F32, kind="ExternalInput").ap())
    cx.mli = nc.dram_tensor("mli", [2, S], F32, kind="ExternalInput").ap()
    with ExitStack() as es:
        tk = TK(nc, es)
        cx.pp = PsumPool(tk, 4)
        mp = PsumPool(tk, 4, "mps")
        cx.mpo = [(mp.t[0], mp.r[0]), (mp.t[1], mp.r[1])]
        cx.mpd = [(mp.t[2], mp.r[2]), (mp.t[3], mp.r[3])]
        cst, cstr = ld_const(tk, "cstt", [128, 640], cx.cst)
        cx.identf, cx.identfr = cst[:, 0:128], cstr
        cx.tri, cx.trir = cst[:, 128:640].rearrange("p (a b) -> p a b", a=2), cstr
        cx.identb = tk.sb("identb", [128, 128], BF16)
        cx.identbr = Res("identb")
        tk.dve(lambda e: e.tensor_copy(out=cx.identb[:, :], in_=cst[:, 0:128]), [cstr], [cx.identbr])
        cx.onesf = tk.sb("onesf", [128, 128], F32)
        cx.onesfr = Res("onesf")
        tk.dve(lambda e: e.memset(cx.onesf[:, :], 1.0), [], [cx.onesfr])
        cx.onesb = tk.sb("onesb", [128, 128], BF16)
        cx.onesbr = Res("onesb")
        tk.dve(lambda e: e.memset(cx.onesb[:, :], 1.0), [], [cx.onesbr])
        cx.one1 = tk.sb("one1", [128, 1], F32)
        cx.one1r = Res("one1")
        tk.dve(lambda e: e.memset(cx.one1[:, :], 1.0), [], [cx.one1r])
        cx.eps1 = tk.sb("eps1", [128, 1], F32)
        cx.eps1r = Res("eps1")
        tk.dve(lambda e: e.memset(cx.eps1[:, :], EPS), [], [cx.eps1r])
        if "L" in which:
            emit_L(tk, cx, S)
        if "R" in which:
            emit_R(tk, cx, S)
        if "S" in which:
            emit_S(tk, cx, S)
        if "M" in which:
            emit_M(tk, cx, S)
        tk.finish()
    return nc


def static_tables(g):
    f = np.float32
    idx = np.arange(256, dtype=np.float64)
    j128 = np.arange(128)
    log_g = np.log1p(-2.0 ** (-5.0 - g))
    rt = np.zeros((128, 773), f)
    for jt in range(2):
        j = jt * 128 + j128
        diff = idx[None, :] - j[:, None]
        rt[:, jt * 256:(jt + 1) * 256] = np.where(diff >= 0, np.exp(log_g * np.maximum(diff, 0)), 0.0)
        rt[:, 768 + jt] = np.exp(log_g * (255.0 - j))
    rt[:, 512:768] = np.exp(log_g * (idx + 1.0))[None, :]
    rt[:, 770] = np.exp(log_g * 256.0)
    cst = np.zeros((128, 640), f)
    cst[:, 0:128] = np.eye(128, dtype=f)
    for jt in range(2):
        j = jt * 128 + j128
        cst[:, 128 + jt * 256:128 + (jt + 1) * 256] = (j[:, None] <= np.arange(256)[None, :]).astype(f)
    mt = np.zeros((128, 4), f)
    mbdc = np.zeros((16, 32), f)
    mli = np.zeros((2, SEQ), f)
    for hh in range(2):
        slope = 2.0 ** (-(2 * g + hh + 1))
        for jt in range(2):
            mt[:, hh * 2 + jt] = slope * (jt * 128 + j128)
        for a in range(16):
            mbdc[:, hh * 16 + a] = -slope * 256.0 * (a - np.arange(16))
        mli[hh] = -slope * (np.arange(SEQ) % 256)
    moh = np.zeros((17, 17, 128), f)
    for n in range(16):
        moh[n, n, :] = 1.0
        moh[16, n, :] = 1.0
    moh[16, 16, :] = 1.0
    mcb = np.zeros((128, 2, 256), f)
    for jt in range(2):
        j = jt * 128 + j128
        mcb[:, jt, :] = np.where(j[:, None] > np.arange(256)[None, :], NEG, 0.0)
    mpm = np.zeros((128, 16, 16), f)
    for a in range(16):
        mpm[:, a, a:] = NEG
    return dict(rt=rt, cst=cst, mt=mt, mbdc=mbdc, mli=mli, moh=moh.reshape(17, -1), mcb=mcb.reshape(128, -1), mpm=mpm.reshape(128, -1))


def col2(v):
    return np.ascontiguousarray(np.asarray(v, np.float32).reshape(2, 128).T)


def x_params(l, g, P, tabs):
    f = np.float32
    d = dict(tabs)
    rt = tabs["rt"].copy()
    rt[:, 771:773] = col2(P["ret_gn_w"][l, g])
    d["rt"] = rt
    lp = np.zeros((128, 16), f)
    sl = slice(g * 256, (g + 1) * 256)
    cw = P["lru_conv_w"][l][:, sl]
    for n in range(2):
        for k in range(4):
            lp[:, n * 4 + k] = cw[k, n * 128:(n + 1) * 128]
    lp[:, 8:10] = col2(P["lru_conv_b"][l][sl])
    lp[:, 10:12] = col2(P["lru_b_a"][l][sl])
    lp[:, 12:14] = col2(P["lru_b_x"][l][sl])
    lp[:, 14:16] = col2(P["lru_lambda"][l][sl])
    d["lp"] = lp
    lw = np.zeros((128, 4, 128), f)
    for n in range(2):
        lw[:, n, :] = P["lru_w_a"][l, 2 * g + n]
        lw[:, 2 + n, :] = P["lru_w_x"][l, 2 * g + n]
    d["lw"] = lw.reshape(128, 512)
    st = np.zeros((128, 32), f)
    chans = [slice(g * 256, g * 256 + 128), slice(g * 256 + 128, g * 256 + 256),
             slice(1024 + g * 128, 1024 + (g + 1) * 128), slice(1536 + g * 128, 1536 + (g + 1) * 128)]
    for t4, ch in enumerate(chans):
        for k in range(4):
            st[:, t4 * 4 + k] = P["ssm_conv_w"][l][k, ch]
        st[:, 16 + t4] = P["ssm_conv_b"][l][ch]
    st[:, 20:22] = col2(np.repeat(P["ssm_d"][l][4 * g:4 * g + 4], 64))
    st[:, 22:24] = col2(P["ssm_norm_w"][l][sl])
    st[:, 24:28] = P["ssm_dt_bias"][l][4 * g:4 * g + 4][None, :]
    st[:, 28:32] = P["ssm_a_log"][l][4 * g:4 * g + 4][None, :]
    d["st"] = st
    return d


def w_in_cols(g):
    B = 1024
    s = lambda base, n=256: list(range(base + g * n, base + (g + 1) * n))
    cols = []
    cols += s(0) + s(B) + s(3 * B)
    cols += s(4 * B) + s(5 * B) + s(7 * B)
    xbc = 9 * B
    cols += s(8 * B) + s(xbc) + s(xbc + 1024, 128) + s(xbc + 1536, 128)
    cols += list(range(xbc + 2048 + 4 * g, xbc + 2048 + 4 * g + 4))
    lbase = xbc + 2048 + 16
    cols += s(lbase) + s(lbase + B)
    cols += s(2 * B) + s(6 * B)
    assert len(cols) == NCOL
    return np.asarray(cols)


_PROGS = {}


def _prog(name):
    if name not in _PROGS:
        if name == "P":
            _PROGS[name] = build_P()
        elif name == "X":
            _PROGS[name] = build_X()
        elif name == "B0":
            _PROGS[name] = build_B(proj=False)
        elif name == "B":
            _PROGS[name] = build_B(proj=True)
        elif name == "BF":
            _PROGS[name] = build_B(proj=True, final=True)
    return _PROGS[name]


def _run(name, in_maps):
    res = run_bass_kernel_spmd(_prog(name), in_maps, core_ids=list(range(NCORES)))
    return res.results


def _kt_layout(a):
    K_, N_ = a.shape
    return np.ascontiguousarray(a.reshape(K_ // 128, 128, N_).transpose(1, 0, 2).reshape(128, -1))


def kernel(**inputs):
    P = {k: np.asarray(v) for k, v in inputs.items()}
    x = P["x"]
    TBK = SEQ // 4
    tabs = [static_tables(g) for g in range(4)]
    cols = [w_in_cols(g) for g in range(4)]
    perm = np.asarray([m * 1024 + g * 256 + j for g in range(4) for m in range(4) for j in range(256)])
    nwl = lambda v: np.ascontiguousarray(np.asarray(v, np.float32).reshape(D_MODEL // 128, 128).T)
    xT = [np.ascontiguousarray(x[c // 4, (c % 4) * TBK:(c % 4 + 1) * TBK, :].T) for c in range(NCORES)]
    r = _run("B0", [{"xT": xT[c], "nw": nwl(P["norm_w"][0])} for c in range(NCORES)])
    hn = [r[c]["hn"] for c in range(NCORES)]
    out = None
    for l in range(DEPTH):
        hT = [np.concatenate([hn[b * 4 + q] for q in range(4)], axis=1) for b in range(BATCH)]
        hTl = [_kt_layout(h) for h in hT]
        wl = [np.ascontiguousarray(P["w_in"][l][:, cols[g]]) for g in range(4)]
        rp = _run("P", [{"hT": hTl[c // 4], "w": wl[c % 4]} for c in range(NCORES)])
        xin = []
        for c in range(NCORES):
            m = x_params(l, c % 4, P, tabs[c % 4])
            m["fm"] = rp[c]["fm"]
            m["tm"] = rp[c]["tm"]
            xin.append(m)
        rx = _run("X", xin)
        yb = [np.concatenate([rx[b * 4 + g]["yT"] for g in range(4)], axis=0) for b in range(BATCH)]
        wo = np.ascontiguousarray(P["w_out"][l][perm, :])
        last = (l == DEPTH - 1)
        nw = nwl(P["final_norm_w"] if last else P["norm_w"][l + 1])
        bin_ = []
        for c in range(NCORES):
            b, q = c // 4, c % 4
            bin_.append({"xT": xT[c], "nw": nw, "yT": _kt_layout(np.ascontiguousarray(yb[b][:, q * TBK:(q + 1) * TBK])), "wo": wo})
        rb = _run("BF" if last else "B", bin_)
        xT = [rb[c]["xn"] for c in range(NCORES)]
        hn = [rb[c]["hn"] for c in range(NCORES)]
    out = np.empty((BATCH, SEQ, D_MODEL), np.float32)
    for c in range(NCORES):
        out[c // 4, (c % 4) * TBK:(c % 4 + 1) * TBK, :] = np.asarray(hn[c], np.float32).T
    return out
```
